# Optimizing a Trainium2 kernel written in Bass

```python
import math
import jax, jax.numpy as jnp
from jax import lax
import numpy as np

D_MODEL = 2048
BATCH = 2
SEQ = 8192
DEPTH = 2

HEAD_DIM = 128
A_HEADS = 4
MOBA_BLOCK = 256
MOBA_TOPK = 3
MOBA_Q_CHUNK = 64
B_HEADS = 4
FOX_Q_BLOCK = 128
CONV_CH = 512
CONV_WIDTH = 31
D_Q_HEADS = 8
D_KV_HEADS = 2
D_HEAD_DIM = 64
WINDOW = 128
N_BUCKETS = 32
MAX_DISTANCE = 1024
N_BIAS_HEADS = A_HEADS + D_Q_HEADS
N_BRANCHES = 4
D_FF = -(-8 * D_MODEL // (3 * 256)) * 256
RMS_EPS = 1e-6
LN_EPS = 1e-5

A_W = A_HEADS * HEAD_DIM
B_W = B_HEADS * HEAD_DIM
D_QW = D_Q_HEADS * D_HEAD_DIM
D_KVW = D_KV_HEADS * D_HEAD_DIM
IN_SPLITS = (A_W, A_W, A_W, B_W, B_W, B_W, B_HEADS, 2 * CONV_CH, D_QW, D_KVW, D_KVW, N_BRANCHES * D_MODEL)
IN_OFFSETS = tuple(int(v) for v in np.cumsum(IN_SPLITS)[:-1])
IN_W = int(sum(IN_SPLITS))

kernel_name = 'hybrid_gated_moba_fox_conformer_swa'


def rms_norm(x, g):
    xf = x.astype(jnp.float32)
    y = xf * lax.rsqrt(jnp.mean(xf * xf, axis=-1, keepdims=True) + RMS_EPS)
    return (y * g.astype(jnp.float32)).astype(x.dtype)


def layer_norm(x, g, b):
    xf = x.astype(jnp.float32)
    mu = jnp.mean(xf, axis=-1, keepdims=True)
    var = jnp.mean(jnp.square(xf - mu), axis=-1, keepdims=True)
    y = (xf - mu) * lax.rsqrt(var + LN_EPS)
    return (y * g.astype(jnp.float32) + b.astype(jnp.float32)).astype(x.dtype)


def t5_bucket(dist):
    max_exact = N_BUCKETS // 2
    d = jnp.maximum(dist, 0)
    log_ratio = jnp.log(jnp.maximum(d, 1).astype(jnp.float32) / max_exact) / math.log(MAX_DISTANCE / max_exact)
    large = max_exact + (log_ratio * (N_BUCKETS - max_exact)).astype(jnp.int32)
    large = jnp.minimum(large, N_BUCKETS - 1)
    return jnp.where(d < max_exact, d, large)


def moba_attention(q, k, v, rel_bias):
    bsz, seq, n_heads, dh = q.shape
    n_blk = -(-seq // MOBA_BLOCK)
    seq_p = n_blk * MOBA_BLOCK
    n_sel = min(MOBA_TOPK, n_blk)
    pad = ((0, 0), (0, seq_p - seq), (0, 0), (0, 0))
    qh = jnp.pad(q, pad).transpose(0, 2, 1, 3)
    k_blocks = jnp.pad(k, pad).transpose(0, 2, 1, 3).reshape(bsz, n_heads, n_blk, MOBA_BLOCK, dh)
    v_blocks = jnp.pad(v, pad).transpose(0, 2, 1, 3).reshape(bsz, n_heads, n_blk, MOBA_BLOCK, dh)
    k_mean = jnp.mean(k_blocks.astype(jnp.float32), axis=3)
    bias_tab = rel_bias.T
    head_ix = jnp.arange(n_heads)[None, :, None, None, None]
    blk_ids = jnp.arange(n_blk)
    offs = jnp.arange(MOBA_BLOCK)
    gather_blocks = jax.vmap(jax.vmap(lambda blocks, idx: blocks[idx]))
    scale = dh ** -0.5

    def one_chunk(ci):
        q0 = ci * MOBA_Q_CHUNK
        qc = lax.dynamic_slice_in_dim(qh, q0, MOBA_Q_CHUNK, axis=2)
        t = q0 + jnp.arange(MOBA_Q_CHUNK)
        own = t // MOBA_BLOCK
        score = jnp.einsum('bhqd,bhnd->bhqn', qc.astype(jnp.float32), k_mean)
        score = jnp.where(blk_ids[None, :] < own[:, None], score, -jnp.inf)
        _, top_i = lax.top_k(score, n_sel)
        own_b = jnp.broadcast_to(own[:, None], top_i.shape[:-1] + (1,))
        blk_idx = jnp.concatenate([top_i, own_b], axis=-1)
        blk_ok = jnp.concatenate([top_i < own[:, None], jnp.ones_like(own_b, dtype=bool)], axis=-1)
        ks = gather_blocks(k_blocks, blk_idx)
        vs = gather_blocks(v_blocks, blk_idx)
        dist = t[:, None, None] - (blk_idx[..., None] * MOBA_BLOCK + offs)
        valid = blk_ok[..., None] & (dist >= 0)
        logits = jnp.einsum('bhqd,bhqnkd->bhqnk', qc, ks, preferred_element_type=jnp.float32) * scale
        logits = logits + bias_tab[head_ix, t5_bucket(dist)].astype(jnp.float32)
        logits = jnp.where(valid, logits, -jnp.inf).reshape(bsz, n_heads, MOBA_Q_CHUNK, -1)
        p = jax.nn.softmax(logits, axis=-1).reshape(valid.shape).astype(vs.dtype)
        return jnp.einsum('bhqnk,bhqnkd->bhqd', p, vs)

    out = lax.map(one_chunk, jnp.arange(seq_p // MOBA_Q_CHUNK))
    out = out.transpose(1, 0, 3, 2, 4).reshape(bsz, seq_p, n_heads * dh)
    return out[:, :seq]


def forgetting_attention(q, k, v, log_f):
    bsz, seq, n_heads, dh = q.shape
    qh = q.transpose(0, 2, 1, 3)
    kh = k.transpose(0, 2, 1, 3)
    vh = v.transpose(0, 2, 1, 3)
    cum = jnp.cumsum(log_f, axis=1).transpose(0, 2, 1)
    kpos = jnp.arange(seq)
    scale = dh ** -0.5

    def one_block(bi):
        q0 = bi * FOX_Q_BLOCK
        qb = lax.dynamic_slice_in_dim(qh, q0, FOX_Q_BLOCK, axis=2)
        cq = lax.dynamic_slice_in_dim(cum, q0, FOX_Q_BLOCK, axis=2)
        t = q0 + jnp.arange(FOX_Q_BLOCK)
        logits = (jnp.einsum('bhqd,bhkd->bhqk', qb, kh, preferred_element_type=jnp.float32) * scale
                  + (cq[..., None] - cum[:, :, None, :]))
        logits = jnp.where(kpos[None, :] <= t[:, None], logits, -jnp.inf)
        p = jax.nn.softmax(logits, axis=-1).astype(vh.dtype)
        return jnp.einsum('bhqk,bhkd->bhqd', p, vh)

    out = lax.map(one_block, jnp.arange(seq // FOX_Q_BLOCK))
    return out.transpose(1, 0, 3, 2, 4).reshape(bsz, seq, n_heads * dh)


def conformer_conv(u, conv_w, conv_b, ln_g, ln_b):
    a, gte = jnp.split(u, 2, axis=-1)
    h = a * jax.nn.sigmoid(gte)
    h = lax.conv_general_dilated(h, conv_w[:, None, :], window_strides=(1,), padding=[(CONV_WIDTH - 1, 0)],
                                 dimension_numbers=('NWC', 'WIO', 'NWC'), feature_group_count=CONV_CH) + conv_b
    return jax.nn.silu(layer_norm(h, ln_g, ln_b))


def sliding_window_attention(q, k, v, sinks, rel_bias):
    bsz, seq, n_q, dh = q.shape
    n_kv = k.shape[2]
    grp = n_q // n_kv
    nb = seq // WINDOW
    qb = q.reshape(bsz, nb, WINDOW, n_kv, grp, dh)

    def band(t):
        prev = jnp.pad(t, ((0, 0), (WINDOW, 0), (0, 0), (0, 0)))[:, :seq]
        return jnp.concatenate([prev.reshape(bsz, nb, WINDOW, n_kv, dh), t.reshape(bsz, nb, WINDOW, n_kv, dh)], axis=2)

    kb, vb = band(k), band(v)
    qi = jnp.arange(WINDOW)[:, None]
    kj = jnp.arange(2 * WINDOW)[None, :]
    dist = qi + WINDOW - kj
    in_win = (dist >= 0) & (dist < WINDOW)
    not_pad = (jnp.arange(nb)[:, None, None] > 0) | (kj >= WINDOW)[None]
    valid = in_win[None] & not_pad
    bias = rel_bias[t5_bucket(dist)].transpose(2, 0, 1).reshape(n_kv, grp, 1, WINDOW, 2 * WINDOW)
    logits = jnp.einsum('bnqhgd,bnkhd->bhgnqk', qb, kb, preferred_element_type=jnp.float32) * (dh ** -0.5)
    logits = jnp.where(valid, logits + bias.astype(jnp.float32), -jnp.inf)
    sink = jnp.broadcast_to(sinks.reshape(n_kv, grp, 1, 1, 1).astype(jnp.float32), logits.shape[:-1] + (1,))
    p = jax.nn.softmax(jnp.concatenate([logits, sink], axis=-1), axis=-1)[..., :-1].astype(vb.dtype)
    out = jnp.einsum('bhgnqk,bnkhd->bnqhgd', p, vb)
    return out.reshape(bsz, seq, n_q * dh)


def hybrid_mixer(h, rel_bias, w_in, fox_b, conv_w, conv_b, conv_g, conv_beta, sinks,
                 w_br_a, w_br_b, w_br_c, w_br_d, w_out):
    bsz, seq, _ = h.shape
    proj = h @ w_in
    aq, ak, av, bq, bk, bv, bf, cu, dq, dk, dv, gl = jnp.split(proj, IN_OFFSETS, axis=-1)

    def heads(t, n):
        return t.reshape(bsz, seq, n, -1)

    ya = moba_attention(heads(aq, A_HEADS), heads(ak, A_HEADS), heads(av, A_HEADS), rel_bias[:, :A_HEADS])
    log_f = jax.nn.log_sigmoid((bf + fox_b).astype(jnp.float32))
    yb = forgetting_attention(heads(bq, B_HEADS), heads(bk, B_HEADS), heads(bv, B_HEADS), log_f)
    yc = conformer_conv(cu, conv_w, conv_b, conv_g, conv_beta)
    yd = sliding_window_attention(heads(dq, D_Q_HEADS), heads(dk, D_KV_HEADS), heads(dv, D_KV_HEADS),
                                  sinks, rel_bias[:, A_HEADS:])
    gates = jax.nn.sigmoid(gl.reshape(bsz, seq, N_BRANCHES, D_MODEL))
    y = (gates[:, :, 0] * (ya @ w_br_a) + gates[:, :, 1] * (yb @ w_br_b)
         + gates[:, :, 2] * (yc @ w_br_c) + gates[:, :, 3] * (yd @ w_br_d))
    return y @ w_out


def swiglu(h, w_gate, w_up, w_down):
    return (jax.nn.silu(h @ w_gate) * (h @ w_up)) @ w_down


def _normal(key, shape, scale):
    return jax.random.normal(key, shape, jnp.float32) * scale


def setup_inputs(seed: int = 0) -> dict:
    key = jax.random.key(seed)
    ks = jax.random.split(key, 24)
    d = D_MODEL
    return {
        'x': _normal(ks[0], (BATCH, SEQ, d), 1.0),
        'c': _normal(ks[1], (BATCH, d), 1.0),
        'rel_bias': _normal(ks[2], (N_BUCKETS, N_BIAS_HEADS), 0.5),
        'w_mod': _normal(ks[3], (DEPTH, d, 6 * d), 0.5 * d ** -0.5),
        'b_mod': _normal(ks[4], (DEPTH, 6 * d), 0.02),
        'mix_norm_pre': 1.0 + _normal(ks[5], (DEPTH, d), 0.05),
        'mix_norm_post': 1.0 + _normal(ks[6], (DEPTH, d), 0.05),
        'w_in': _normal(ks[7], (DEPTH, d, IN_W), d ** -0.5),
        'fox_bias': jax.random.uniform(ks[8], (DEPTH, B_HEADS), jnp.float32, minval=1.0, maxval=4.0),
        'conv_w': _normal(ks[9], (DEPTH, CONV_WIDTH, CONV_CH), CONV_WIDTH ** -0.5),
        'conv_b': _normal(ks[10], (DEPTH, CONV_CH), 0.02),
        'conv_ln_g': 1.0 + _normal(ks[11], (DEPTH, CONV_CH), 0.05),
        'conv_ln_b': _normal(ks[12], (DEPTH, CONV_CH), 0.02),
        'sinks': _normal(ks[13], (DEPTH, D_Q_HEADS), 1.0),
        'w_branch_a': _normal(ks[14], (DEPTH, A_W, d), A_W ** -0.5),
        'w_branch_b': _normal(ks[15], (DEPTH, B_W, d), B_W ** -0.5),
        'w_branch_c': _normal(ks[16], (DEPTH, CONV_CH, d), CONV_CH ** -0.5),
        'w_branch_d': _normal(ks[17], (DEPTH, D_QW, d), D_QW ** -0.5),
        'w_out': _normal(ks[18], (DEPTH, d, d), d ** -0.5),
        'ffn_norm_pre': 1.0 + _normal(ks[19], (DEPTH, d), 0.05),
        'ffn_norm_post': 1.0 + _normal(ks[20], (DEPTH, d), 0.05),
        'w_ffn_gate': _normal(ks[21], (DEPTH, d, D_FF), d ** -0.5),
        'w_ffn_up': _normal(ks[22], (DEPTH, d, D_FF), d ** -0.5),
        'w_ffn_down': _normal(ks[23], (DEPTH, D_FF, d), D_FF ** -0.5),
    }


def reference(x, c, rel_bias, w_mod, b_mod, mix_norm_pre, mix_norm_post, w_in, fox_bias,
              conv_w, conv_b, conv_ln_g, conv_ln_b, sinks, w_branch_a, w_branch_b, w_branch_c,
              w_branch_d, w_out, ffn_norm_pre, ffn_norm_post, w_ffn_gate, w_ffn_up, w_ffn_down):
    c_act = jax.nn.silu(c)
    for l in range(DEPTH):
        mod = c_act @ w_mod[l] + b_mod[l]
        sh_m, sc_m, gt_m, sh_f, sc_f, gt_f = jnp.split(mod[:, None, :], 6, axis=-1)
        h = rms_norm(x, mix_norm_pre[l]) * (1.0 + sc_m) + sh_m
        y = hybrid_mixer(h, rel_bias, w_in[l], fox_bias[l], conv_w[l], conv_b[l], conv_ln_g[l], conv_ln_b[l],
                         sinks[l], w_branch_a[l], w_branch_b[l], w_branch_c[l], w_branch_d[l], w_out[l])
        x = x + gt_m * rms_norm(y, mix_norm_post[l])
        h = rms_norm(x, ffn_norm_pre[l]) * (1.0 + sc_f) + sh_f
        y = swiglu(h, w_ffn_gate[l], w_ffn_up[l], w_ffn_down[l])
        x = x + gt_f * rms_norm(y, ffn_norm_post[l])
    return x
```

```python
import contextlib
import math
import numpy as np
import ml_dtypes
import concourse.bass as bass
import concourse.mybir as mybir
from concourse.bass_utils import run_bass_kernel_spmd

F32 = mybir.dt.float32
BF16 = mybir.dt.bfloat16
AF = mybir.ActivationFunctionType
ALU = mybir.AluOpType
AX = mybir.AxisListType
NPBF = ml_dtypes.bfloat16

D = 2048
SEQ = 8192
NB = 2
DEPTH = 2
IN_W = 13060
DFF = 5632
TOK = 2048
NEG = -32768.0
RMS_EPS = 1e-6
LN_EPS = 1e-5
O_AQ, O_AK, O_AV = 0, 512, 1024
O_BQ, O_BK, O_BV = 1536, 2048, 2560
O_BF = 3072
O_CU = 3076
O_DQ = 4100
O_DK = 4612
O_DV = 4740
O_GL = 4868


class Prog:
    COMPUTE = ("tensor", "vector", "scalar", "gpsimd")
    NSLOT = 6

    def __init__(self, nc):
        self.nc = nc
        self.ops = {e: [] for e in ("tensor", "vector", "scalar", "gpsimd", "sync")}
        self.cnt = {e: 0 for e in self.COMPUTE}
        self.dcnt = {e: 0 for e in ("sync", "scalar", "gpsimd")}
        self.last_w = {}
        self.readers = {}
        self.waited = {e: {} for e in self.ops}
        self.semnames = set()
        self.out_tokens = []

    def _deps(self, eng, reads, writes):
        deps = []
        for k in reads:
            w = self.last_w.get(k)
            if w is not None:
                deps.append(("raw", w))
        for k in writes:
            w = self.last_w.get(k)
            if w is not None:
                deps.append(("waw", w))
            for r in self.readers.get(k, ()):
                deps.append(("war", r))
        best = {}
        for kind, (peng, sem, val, isdma) in deps:
            if peng == eng and not isdma:
                if eng == "tensor" or kind != "raw":
                    continue
            if self.waited[eng].get(sem, 0) >= val:
                continue
            best[sem] = max(best.get(sem, 0), val)
        for s, v in best.items():
            self.waited[eng][s] = v
        return list(best.items())

    def _commit(self, tok, reads, writes):
        for k in reads:
            self.readers.setdefault(k, []).append(tok)
        for k in writes:
            self.last_w[k] = tok
            self.readers[k] = []

    def op(self, eng, fn, reads=(), writes=(), sig=True):
        waits = self._deps(eng, reads, writes)
        sem = "c_" + eng
        self.semnames.add(sem)
        if sig:
            self.cnt[eng] += 1
            tok = (eng, sem, self.cnt[eng], False)
            inc = (sem, 1)
        else:
            tok = (eng, sem, self.cnt[eng] + 1, False)
            inc = None
        self.ops[eng].append((fn, waits, inc))
        self._commit(tok, reads, writes)
        return tok

    def dma(self, eng, fn, reads=(), writes=(), is_out=False):
        waits = dict(self._deps(eng, reads, writes))
        i = self.dcnt[eng]
        self.dcnt[eng] += 1
        nslot = 2 if eng == "gpsimd" else self.NSLOT
        slot = i % nslot
        rnd = i // nslot
        sem = "d_%s_%d" % (eng, slot)
        self.semnames.add(sem)
        if rnd > 0 and self.waited[eng].get(sem, 0) < 16 * rnd:
            waits[sem] = max(waits.get(sem, 0), 16 * rnd)
            self.waited[eng][sem] = waits[sem]
        tok = (eng, sem, 16 * (rnd + 1), True)
        self.ops[eng].append((fn, list(waits.items()), (sem, 16)))
        self._commit(tok, reads, writes)
        if is_out:
            self.out_tokens.append(tok)
        return tok

    def emit(self):
        nc = self.nc
        for e in self.COMPUTE:
            assert self.cnt[e] < 60000, (e, self.cnt[e])
        with contextlib.ExitStack() as st:
            sems = {}
            for name in sorted(self.semnames):
                sems[name] = st.enter_context(nc.semaphore(name))
            finals = {}
            for (_, sem, val, _) in self.out_tokens:
                finals[sem] = max(finals.get(sem, 0), val)
            block = st.enter_context(nc.Block())

            def mk(engname):
                lst = self.ops[engname]

                def body(e):
                    for fn, waits, inc in lst:
                        for s, v in waits:
                            e.wait_ge(sems[s], v)
                        ins = fn(e)
                        if inc is not None:
                            ins.then_inc(sems[inc[0]], inc[1])
                    if engname == "sync":
                        for s, v in finals.items():
                            e.wait_ge(sems[s], v)
                return body

            for engname in ("sync", "tensor", "vector", "scalar", "gpsimd"):
                if self.ops[engname] or engname == "sync":
                    getattr(block, engname)(mk(engname))


class Ctx:
    def __init__(self):
        self.nc = bass.Bass("TRN2", target_bir_lowering=False)
        self.P = Prog(self.nc)
        self.st = contextlib.ExitStack()
        self.rr = 0

    def dram_in(self, name, shape, dt):
        return self.nc.dram_tensor(name, list(shape), dt, kind="ExternalInput").ap()

    def dram_out(self, name, shape, dt):
        return self.nc.dram_tensor(name, list(shape), dt, kind="ExternalOutput").ap()

    def dram_tmp(self, name, shape, dt):
        return self.nc.dram_tensor(name, list(shape), dt).ap()

    def sb(self, name, shape, dt):
        return self.st.enter_context(self.nc.sbuf_tensor(name, list(shape), dt))

    def ps(self, name, shape, dt=F32):
        return self.st.enter_context(self.nc.psum_tensor(name, list(shape), dt))

    def finish(self):
        self.P.emit()
        self.st.close()
        return self.nc

    def alt(self):
        self.rr += 1
        return "vector" if self.rr % 2 else "scalar"

    def copy(self, eng, out, in_, reads, writes, scale=None):
        P = self.P
        if eng == "scalar":
            if scale is None:
                P.op("scalar", lambda e: e.copy(out=out, in_=in_), reads=reads, writes=writes)
            else:
                P.op("scalar", lambda e: e.activation(out=out, in_=in_, func=AF.Copy, scale=float(scale)), reads=reads, writes=writes)
        else:
            if scale is None:
                P.op(eng, lambda e: e.tensor_copy(out=out, in_=in_), reads=reads, writes=writes)
            else:
                P.op(eng, lambda e: e.tensor_scalar(out=out, in0=in_, scalar1=float(scale), scalar2=None, op0=ALU.mult), reads=reads, writes=writes)

    def make_ident(self, name, dt):
        t32 = self.sb(name + "_f", [128, 128], F32)
        P = self.P
        P.op("gpsimd", lambda e: e.memset(t32[:], 1.0), writes=[name + "_f"])
        P.op("gpsimd", lambda e: e.affine_select(out=t32[:], in_=t32[:], pattern=[[-1, 128]], compare_op=ALU.is_equal,
                                                 fill=0.0, base=0, channel_multiplier=1), reads=[name + "_f"], writes=[name + "_f"])
        if dt == F32:
            return t32, name + "_f"
        t = self.sb(name, [128, 128], dt)
        P.op("vector", lambda e: e.tensor_copy(out=t[:], in_=t32[:]), reads=[name + "_f"], writes=[name])
        return t, name

    def make_const(self, name, shape, dt, val):
        t = self.sb(name, shape, dt)
        self.P.op("gpsimd", lambda e: e.memset(t[:], float(val)), writes=[name])
        return t


def wview(w_ap, col0, ncols, kch):
    return w_ap.rearrange("(k p) n -> p k n", p=128)[:, 0:kch, col0:col0 + ncols]


def emit_mod(cx, cT_ap, wmod_ap, ncols, bmod_sb, bmod_key, out_sb, out_key, wbufs, psum, pskey, bw=512):
    P = cx.P
    c32 = cx.sb("mod_c32", [128, 16], F32)
    cb = cx.sb("mod_cb", [128, 16], BF16)
    P.dma("sync", lambda e: e.dma_start(out=c32[:], in_=cT_ap), writes=["mod_c32"])
    P.op("scalar", lambda e: e.activation(out=cb[:], in_=c32[:], func=AF.Silu), reads=["mod_c32"], writes=["mod_cb"])
    nblk = ncols // bw
    for blk in range(nblk):
        wt, wk = wbufs[blk % len(wbufs)]
        P.dma("gpsimd", lambda e, wt=wt, blk=blk: e.dma_start(out=wt, in_=wview(wmod_ap, blk * bw, bw, 16)), writes=[wk])
        for fc in range(bw // 128):
            col = blk * (bw // 128) + fc
            for k in range(16):
                P.op("tensor", lambda e, wt=wt, fc=fc, k=k, col=col: e.matmul(psum[:, col:col + 1], lhsT=wt[:, k, fc * 128:(fc + 1) * 128],
                                                                               rhs=cb[:, k:k + 1], start=(k == 0), stop=(k == 15)),
                     reads=[wk, "mod_cb"], writes=[pskey], sig=(k == 15))
    nch = ncols // 128
    P.op("vector", lambda e: e.tensor_tensor(out=out_sb[:, 0:nch], in0=psum[:, 0:nch], in1=bmod_sb, op=ALU.add),
         reads=[pskey, bmod_key], writes=[out_key])


def emit_rmsnorm_mod(cx, xsrc, ntok, A_sb, A_key, B_sb, B_key, out_hT, out_key, ones_bf, ps_stat, ps_key, tag, x_keep=None):
    P = cx.P
    xs = cx.sb(tag + "_xs", [128, 16, 512], F32)
    sq = [cx.sb(tag + "_sq%d" % i, [128, 512], BF16) for i in range(2)]
    rstd = cx.sb(tag + "_rstd", [128, 512], F32)
    tmp = [cx.sb(tag + "_tmp%d" % i, [128, 512], F32) for i in range(2)]
    for tc in range(ntok // 512):
        t0 = tc * 512
        for k in range(16):
            P.dma("sync", lambda e, k=k, t0=t0: e.dma_start(out=xs[:, k, :], in_=xsrc[k * 128:(k + 1) * 128, t0:t0 + 512]),
                  writes=[tag + "_xs%d" % k])
        for k in range(16):
            s = sq[k % 2]
            sk = tag + "_sq%d" % (k % 2)
            P.op("scalar", lambda e, s=s, k=k: e.activation(out=s[:], in_=xs[:, k, :], func=AF.Square), reads=[tag + "_xs%d" % k], writes=[sk])
            P.op("tensor", lambda e, s=s, k=k: e.matmul(ps_stat[:], lhsT=ones_bf[:], rhs=s[:], start=(k == 0), stop=(k == 15)),
                 reads=[sk, "ones_bf"], writes=[ps_key])
        P.op("scalar", lambda e: e.activation(out=rstd[:], in_=ps_stat[:], func=AF.Sqrt, bias=RMS_EPS, scale=1.0 / D), reads=[ps_key], writes=[tag + "_rstd"])
        P.op("vector", lambda e: e.reciprocal(out=rstd[:], in_=rstd[:]), reads=[tag + "_rstd"], writes=[tag + "_rstd"])
        for k in range(16):
            t = tmp[k % 2]
            tk = tag + "_tmp%d" % (k % 2)
            P.op("vector", lambda e, t=t, k=k: e.tensor_tensor(out=t[:], in0=xs[:, k, :], in1=rstd[:], op=ALU.mult),
                 reads=[tag + "_xs%d" % k, tag + "_rstd"], writes=[tk])
            P.op("scalar", lambda e, t=t, k=k, t0=t0: e.activation(out=out_hT[:, k, t0:t0 + 512], in_=t[:], func=AF.Identity,
                                                                  scale=A_sb[:, k:k + 1], bias=B_sb[:, k:k + 1]),
                 reads=[tk, A_key, B_key], writes=[out_key + "%d" % k])


def build_A():
    cx = Ctx()
    P = cx.P
    xT = cx.dram_in("xT", [D, TOK], F32)
    cT = cx.dram_in("cT", [128, 16], F32)
    wmod = cx.dram_in("wmodA", [D, 4096], F32)
    vec = cx.dram_in("vecA", [128, 48], F32)
    w_in = cx.dram_in("w_in", [D, IN_W], F32)
    o_fm = cx.dram_out("o_fm", [25, 128, TOK], BF16)
    o_bf = cx.dram_out("o_bf", [4, TOK], F32)
    o_tm = cx.dram_out("o_tm", [TOK, 1152], BF16)
    o_hT = cx.dram_out("o_hT", [16, 128, TOK], BF16)

    ones_bf = cx.make_const("ones_bf", [128, 128], BF16, 1.0)
    vecs = cx.sb("vecs", [128, 48], F32)
    P.dma("sync", lambda e: e.dma_start(out=vecs[:], in_=vec), writes=["vecs"])
    modT = cx.sb("modT", [128, 32], F32)
    wb = [(cx.sb("wb%d" % i, [128, 16, 512], BF16), "wb%d" % i) for i in range(2)]
    ps_mod = cx.ps("ps_mod", [128, 512])
    emit_mod(cx, cT, wmod, 4096, vecs[:, 0:32], "vecs", modT, "modT", [(w[:], k) for (w, k) in wb], ps_mod, "ps_mod")
    Acoef = cx.sb("Acoef", [128, 16], F32)
    P.op("vector", lambda e: e.tensor_scalar(out=Acoef[:], in0=modT[:, 16:32], scalar1=1.0, scalar2=None, op0=ALU.add), reads=["modT"], writes=["Acoef"])
    P.op("vector", lambda e: e.tensor_tensor(out=Acoef[:], in0=Acoef[:], in1=vecs[:, 32:48], op=ALU.mult), reads=["Acoef", "vecs"], writes=["Acoef"])
    hT = cx.sb("hT", [128, 16, TOK], BF16)
    emit_rmsnorm_mod(cx, xT, TOK, Acoef, "Acoef", modT, "modT", hT, "hT", ones_bf, ps_mod, "ps_mod", "nA")
    hkeys = ["hT%d" % k for k in range(16)]
    for k in range(16):
        P.dma("sync", lambda e, k=k: e.dma_start(out=o_hT[k], in_=hT[:, k, :]), reads=["hT%d" % k], is_out=True)

    psA = [cx.ps("psA%d" % i, [128, 512]) for i in range(4)]
    stage = [cx.sb("stage%d" % i, [128, TOK], BF16) for i in range(2)]
    sig = cx.sb("sig", [128, TOK], F32)
    SC128 = 128.0 ** -0.5
    SC64 = 64.0 ** -0.5
    jobs = []
    for i in range(4):
        jobs.append((i, O_AQ + 128 * i, SC128, "plain"))
    for i in range(4):
        jobs.append((4 + i, O_AK + 128 * i, None, "plain"))
    for i in range(4):
        jobs.append((8 + i, O_BQ + 128 * i, SC128, "plain"))
    for i in range(4):
        jobs.append((12 + i, O_BK + 128 * i, None, "plain"))
    for i in range(4):
        jobs.append((None, O_CU + 512 + 128 * i, None, "gate"))
        jobs.append((16 + i, O_CU + 128 * i, None, "glu"))
    for i in range(4):
        jobs.append((20 + i, O_DQ + 128 * i, SC64, "plain"))
    jobs.append((24, O_DK, None, "plain"))
    nj = 0
    for (oc, col, scale, mode) in jobs:
        wt, wk = wb[nj % 2]
        st_t = stage[nj % 2]
        sk = "stage%d" % (nj % 2)
        nj += 1
        P.dma("gpsimd", lambda e, wt=wt, col=col: e.dma_start(out=wt[:, :, 0:128], in_=wview(w_in, col, 128, 16)), writes=[wk])
        for tc in range(4):
            ps = psA[tc]
            pk = "psA%d" % tc
            for k in range(16):
                P.op("tensor", lambda e, ps=ps, wt=wt, k=k, tc=tc: e.matmul(ps[:], lhsT=wt[:, k, 0:128], rhs=hT[:, k, tc * 512:(tc + 1) * 512],
                                                                             start=(k == 0), stop=(k == 15)),
                     reads=[wk, hkeys[k]], writes=[pk], sig=(k == 15))
            sl = slice(tc * 512, (tc + 1) * 512)
            if mode == "gate":
                P.op("scalar", lambda e, ps=ps, sl=sl: e.activation(out=sig[:, sl], in_=ps[:], func=AF.Sigmoid), reads=[pk], writes=["sig%d" % tc])
            elif mode == "glu":
                P.op("vector", lambda e, ps=ps, sl=sl, st_t=st_t: e.tensor_tensor(out=st_t[:, sl], in0=ps[:], in1=sig[:, sl], op=ALU.mult),
                     reads=[pk, "sig%d" % tc], writes=[sk])
            else:
                cx.copy(cx.alt(), st_t[:, sl], ps[:], [pk], [sk], scale=scale)
        if oc is not None:
            P.dma("sync", lambda e, oc=oc, st_t=st_t: e.dma_start(out=o_fm[oc], in_=st_t[:]), reads=[sk], is_out=True)
    wt, wk = wb[nj % 2]
    nj += 1
    bfs = cx.sb("bfs", [4, TOK], F32)
    P.dma("gpsimd", lambda e, wt=wt: e.dma_start(out=wt[:, :, 0:4], in_=wview(w_in, O_BF, 4, 16)), writes=[wk])
    for tc in range(4):
        ps = psA[tc]
        pk = "psA%d" % tc
        for k in range(16):
            P.op("tensor", lambda e, ps=ps, k=k, tc=tc, wt=wt: e.matmul(ps[0:4, :], lhsT=wt[:, k, 0:4], rhs=hT[:, k, tc * 512:(tc + 1) * 512],
                                                               start=(k == 0), stop=(k == 15)),
                 reads=[wk, hkeys[k]], writes=[pk], sig=(k == 15))
        P.op("vector", lambda e, ps=ps, tc=tc: e.tensor_copy(out=bfs[:, tc * 512:(tc + 1) * 512], in_=ps[0:4, :]), reads=[pk], writes=["bfs"])
    P.dma("sync", lambda e: e.dma_start(out=o_bf, in_=bfs[:]), reads=["bfs"], is_out=True)
    stm = [cx.sb("stm%d" % i, [128, 512], BF16) for i in range(2)]
    ntm = 0
    for (col, ncol, ocol) in ((O_AV, 512, 0), (O_BV, 512, 512), (O_DV, 128, 1024)):
        wt, wk = wb[nj % 2]
        nj += 1
        P.dma("gpsimd", lambda e, wt=wt, col=col, ncol=ncol: e.dma_start(out=wt[:, :, 0:ncol], in_=wview(w_in, col, ncol, 16)), writes=[wk])
        for tt in range(16):
            ps = psA[tt % 4]
            pk = "psA%d" % (tt % 4)
            for k in range(16):
                P.op("tensor", lambda e, ps=ps, wt=wt, k=k, tt=tt, ncol=ncol: e.matmul(ps[:, 0:ncol], lhsT=hT[:, k, tt * 128:(tt + 1) * 128], rhs=wt[:, k, 0:ncol],
                                                                                     start=(k == 0), stop=(k == 15)),
                     reads=[wk, hkeys[k]], writes=[pk], sig=(k == 15))
            s = stm[ntm % 2]
            sk = "stm%d" % (ntm % 2)
            ntm += 1
            cx.copy(cx.alt(), s[:, 0:ncol], ps[:, 0:ncol], [pk], [sk])
            P.dma("sync", lambda e, s=s, tt=tt, ncol=ncol, ocol=ocol: e.dma_start(out=o_tm[tt * 128:(tt + 1) * 128, ocol:ocol + ncol], in_=s[:, 0:ncol]),
                  reads=[sk], is_out=True)
    return cx.finish()


def colmajor(v):
    return np.ascontiguousarray(np.asarray(v).reshape(-1, 128).T)


def inputs_A(d, l, b, xT):
    vec = np.concatenate([colmajor(d['b_mod'][l][0:4096]), colmajor(d['mix_norm_pre'][l])], axis=1).astype(np.float32)
    return {
        "xT": np.ascontiguousarray(xT, dtype=np.float32),
        "cT": colmajor(d['c'][b]).astype(np.float32),
        "wmodA": np.ascontiguousarray(d['w_mod'][l][:, 0:4096]),
        "vecA": np.ascontiguousarray(vec),
        "w_in": np.ascontiguousarray(d['w_in'][l]),
    }


def MM(P, out, lhsT, rhs, start, stop, reads, writes, sig=True):
    P.op("tensor", lambda e: e.matmul(out, lhsT=lhsT, rhs=rhs, start=start, stop=stop), reads=reads, writes=writes, sig=sig)


def TR(P, out, in_, ident, reads, writes):
    P.op("tensor", lambda e: e.transpose(out=out, in_=in_, identity=ident), reads=reads, writes=writes)


def ACT(P, out, in_, func, reads, writes, bias=0.0, scale=1.0):
    P.op("scalar", lambda e: e.activation(out=out, in_=in_, func=func, bias=bias, scale=scale), reads=reads, writes=writes)


def TT(P, eng, out, in0, in1, op, reads, writes):
    P.op(eng, lambda e: e.tensor_tensor(out=out, in0=in0, in1=in1, op=op), reads=reads, writes=writes)


def TS(P, eng, out, in0, s1, op0, reads, writes, s2=None, op1=None):
    if op1 is None:
        P.op(eng, lambda e: e.tensor_scalar(out=out, in0=in0, scalar1=s1, scalar2=None, op0=op0), reads=reads, writes=writes)
    else:
        P.op(eng, lambda e: e.tensor_scalar(out=out, in0=in0, scalar1=s1, scalar2=s2, op0=op0, op1=op1), reads=reads, writes=writes)


def CP(P, eng, out, in_, reads, writes):
    if eng == "scalar":
        P.op("scalar", lambda e: e.copy(out=out, in_=in_), reads=reads, writes=writes)
    else:
        P.op(eng, lambda e: e.tensor_copy(out=out, in_=in_), reads=reads, writes=writes)


def MEMSET(P, eng, ap, val, writes):
    P.op(eng, lambda e: e.memset(ap, float(val)), writes=writes)


def DMA(P, q, out, in_, reads=(), writes=(), is_out=False):
    P.dma(q, lambda e: e.dma_start(out=out, in_=in_), reads=reads, writes=writes, is_out=is_out)


def revcols(t_ap_col, n, extra=None):
    dims = [list(t_ap_col.ap[0])]
    if extra is not None:
        dims.append(list(extra))
    dims.append([-1, n])
    return bass.AP(t_ap_col.tensor, t_ap_col.offset, dims)


TW = 1792
TL = TW + 127
TDMAX = TW - 1 - 384


def t5_bucket_np(dist):
    d = np.maximum(dist, 0)
    lr = np.log(np.maximum(d, 1).astype(np.float32) / np.float32(16)) / np.float32(math.log(1024 / 16))
    large = 16 + (lr * np.float32(16)).astype(np.int32)
    large = np.minimum(large, 31)
    return np.where(d < 16, d, large)


def toeplitz_consts():
    dvals = TDMAX - np.arange(TL)
    bk = t5_bucket_np(dvals)
    oh = np.zeros((2, 33, TL), np.float32)
    for v, valid in enumerate((dvals >= 0, (dvals >= 0) & (dvals < 128))):
        oh[v, bk[valid], np.nonzero(valid)[0]] = 1.0
        oh[v, 32, np.nonzero(~valid)[0]] = 1.0
    dd = np.arange(0, 4096)
    b = t5_bucket_np(dd)
    thr31 = int(np.nonzero(b < 31)[0].max() + 1)
    return oh, thr31


def build_B():
    cx = Ctx()
    P = cx.P
    S_ = SEQ
    NT = S_ // 128
    _, THR31 = toeplitz_consts()
    aqT = cx.dram_in("aqT", [128, S_], BF16)
    akT = cx.dram_in("akT", [128, S_], BF16)
    av = cx.dram_in("av", [S_, 128], BF16)
    bqT = cx.dram_in("bqT", [128, S_], BF16)
    bkT = cx.dram_in("bkT", [128, S_], BF16)
    bv = cx.dram_in("bv", [S_, 128], BF16)
    bfr = cx.dram_in("bfr", [1, S_], F32)
    hgT = cx.dram_in("hgT", [128, S_], BF16)
    dqT = cx.dram_in("dqT", [64, 2, S_], BF16)
    dkT = cx.dram_in("dkT", [64, S_], BF16)
    dv = cx.dram_in("dv", [S_, 64], BF16)
    Rsel = cx.dram_in("Rsel", [33, 4], F32)
    OH = cx.dram_in("OH", [2, 33, TL], F32)
    cst = cx.dram_in("cst", [128, 8], F32)
    convw = cx.dram_in("convw", [128, 32], F32)
    yaT = cx.dram_out("yaT", [128, S_], BF16)
    ybT = cx.dram_out("ybT", [128, S_], BF16)
    ydT = cx.dram_out("ydT", [128, S_], BF16)
    ycT = cx.dram_out("ycT", [128, S_], F32)
    Fd = cx.dram_tmp("Fd", [4, 2, TL], F32)
    cps = cx.dram_tmp("cps", [S_], F32)
    cq3 = cx.dram_tmp("cq3", [3, S_], BF16)

    GA = cx.sb("GA", [128, 2 * S_], BF16)
    GB = cx.sb("GB", [128, S_], BF16)
    GC = cx.sb("GC", [128, S_], BF16)
    GD = cx.sb("GD", [128, 2 * S_], BF16)
    GE = cx.sb("GE", [128, S_ + 32], BF16)
    ident_f, _ = cx.make_ident("ident", F32)
    ident_b = cx.sb("ident_b", [128, 128], BF16)
    CP(P, "vector", ident_b[:], ident_f[:], ["ident_f"], ["ident_b"])
    ones_b = cx.make_const("ones_b", [128, 128], BF16, 1.0)
    csts = cx.sb("csts", [128, 8], F32)
    DMA(P, "sync", csts[:], cst, writes=["csts"])
    cw = cx.sb("cw", [128, 32], F32)
    DMA(P, "sync", cw[:], convw, writes=["cw"])
    psS = [cx.ps("psS%d" % i, [128, 512]) for i in range(2)]
    psO = cx.ps("psO", [128, 512])
    psD = cx.ps("psD", [128, 512])
    psM = cx.ps("psM", [128, 512])
    psC = [cx.ps("psC%d" % i, [128, 512]) for i in range(2)]
    Pb = [cx.sb("Pb%d" % i, [128, 512], BF16) for i in range(3)]
    rec = cx.sb("rec", [128, 512], F32)
    ost = [cx.sb("ost%d" % i, [128, 512], BF16) for i in range(2)]
    ostf = [cx.sb("ostf%d" % i, [128, 512], F32) for i in range(2)]

    rs = cx.sb("rs", [33, 4], F32)
    ohs = GC[0:33, :].bitcast(F32)[:, 0:2 * TL].rearrange("p (v l) -> p v l", v=2)
    DMA(P, "sync", rs[:], Rsel, writes=["rs"])
    for v in range(2):
        DMA(P, "sync", ohs[:, v, :], OH[v], writes=["GC"])
    fsb = GE[0:4, 0:8192].bitcast(F32)[:, 0:2 * TL].rearrange("p (v l) -> p v l", v=2)
    for v in range(2):
        for c0 in range(0, TL, 512):
            n = min(512, TL - c0)
            MM(P, psM[0:4, 0:n], rs[:, :], ohs[:, v, c0:c0 + n], True, True, ["rs", "GC"], ["psM"])
            CP(P, "vector", fsb[:, v, c0:c0 + n], psM[0:4, 0:n], ["psM"], ["GE"])
    DMA(P, "sync", Fd, fsb, reads=["GE"], writes=["Fd"])
    ttmp = GB[:, 0:2 * TW].bitcast(F32)
    tabs = {}
    for name, (row, v) in (("TA", (0, 0)), ("TC", (3, 0)), ("TD0", (1, 1)), ("TD1", (2, 1))):
        if name in ("TA", "TC"):
            t = cx.sb(name, [128, TW], BF16)
            dst = t[:]
        else:
            if "TD" not in tabs:
                tabs["TD"] = cx.sb("TD", [128, 2, TW], BF16)
            dst = tabs["TD"][:, int(name[2]), :]
            t = tabs["TD"]
        src = bass.AP(Fd.tensor, (row * 2 + v) * TL, [[1, 128], [1, TW]])
        DMA(P, "sync", ttmp, src, reads=["Fd"], writes=["GB"])
        CP(P, "vector", dst, ttmp, ["GB"], [name if name in ("TA", "TC") else "TD"])
        tabs[name] = t
    TA, TC, TD = tabs["TA"], tabs["TC"], tabs["TD"]

    def trev(tab, u0, n):
        c = TW - 1 - u0
        return revcols(tab[:, c:c + 1], n)

    dg = cx.sb("dg", [128, 31, 128], BF16)
    for w in range(31):
        TS(P, "vector", dg[:, w, :], ident_f[:], cw[:, w:w + 1], ALU.mult, ["ident_f", "cw"], ["dg"])
    MEMSET(P, "gpsimd", GE[:, 0:30], 0.0, ["GE"])
    DMA(P, "sync", GE[:, 30:30 + S_], hgT, writes=["GE"])
    for c in range(S_ // 512):
        ps = psC[c % 2]
        pk = "psC%d" % (c % 2)
        for w in range(31):
            MM(P, ps[:], dg[:, w, :], GE[:, 512 * c + w:512 * c + w + 512], w == 0, w == 30, ["dg", "GE"], [pk], sig=(w == 30))
        of = ostf[c % 2]
        ok = "ostf%d" % (c % 2)
        ACT(P, of[:], ps[:], AF.Identity, [pk, "cw"], [ok], bias=cw[:, 31:32])
        DMA(P, "sync", ycT[:, 512 * c:512 * c + 512], of[:], reads=[ok], is_out=True)

    dq = GA[0:64, :].rearrange("p (h t) -> p h t", h=2)
    DMA(P, "sync", dq, dqT, writes=["GA"])
    DMA(P, "sync", GB[0:64, :], dkT, writes=["GB"])
    vdp = GD[:, :].rearrange("p (t h d) -> p t h d", h=2, d=128)
    MEMSET(P, "gpsimd", GD[:, :], 0.0, ["GD"])
    dvv = dv.rearrange("(t p) d -> p t d", p=128)
    DMA(P, "sync", vdp[:, :, 0, 0:64], dvv, writes=["GD"])
    DMA(P, "sync", vdp[:, :, 1, 64:128], dvv, writes=["GD"])
    onesp = cx.sb("onesp", [128, 2, 128], BF16)
    MEMSET(P, "gpsimd", onesp[:], 0.0, ["onesp"])
    MEMSET(P, "gpsimd", onesp[:, 0, 0:64], 1.0, ["onesp"])
    MEMSET(P, "gpsimd", onesp[:, 1, 64:128], 1.0, ["onesp"])
    esink = cx.sb("esink", [128, 1], F32)
    ACT(P, esink[0:64, :], csts[0:64, 2:3], AF.Exp, ["csts"], ["esink"])
    ACT(P, esink[64:128, :], csts[64:128, 3:4], AF.Exp, ["csts"], ["esink"])
    ydst = cx.sb("ydst", [128, 512], BF16)
    npb = 0
    for i in range(NT):
        kks = [i - 1, i] if i > 0 else [i]
        mm_first = True
        for kk in kks:
            dlt = 128 * (i - kk)
            S = psS[npb % 2]
            sk = "psS%d" % (npb % 2)
            pb = Pb[npb % 3]
            pbk = "Pb%d" % (npb % 3)
            npb += 1
            Sv = S[:, 0:256].rearrange("p (h t) -> p h t", h=2)
            MM(P, Sv, GB[0:64, kk * 128:(kk + 1) * 128], dq[:, :, i * 128:(i + 1) * 128], True, False, ["GA", "GB"], [sk], sig=False)
            u0 = dlt + 384
            c = TW - 1 - u0
            rv = revcols(TD[:, 0, c:c + 1], 128, extra=[TW, 2])
            MM(P, Sv, ident_b[:], rv, False, True, ["ident_b", "TD"], [sk])
            ACT(P, pb[:, 0:256], S[:, 0:256], AF.Exp, [sk], [pbk])
            for hh in range(2):
                last = (kk == kks[-1]) and hh == 1
                MM(P, psC[0][:, 0:128], vdp[:, kk, hh, :], pb[:, hh * 128:(hh + 1) * 128], mm_first, last, ["GD", pbk], ["psC0"])
                MM(P, psC[1][:, 0:128], onesp[:, hh, :], pb[:, hh * 128:(hh + 1) * 128], mm_first, last, ["onesp", pbk], ["psC1"])
                mm_first = False
        TS(P, "vector", rec[:, 0:128], psC[1][:, 0:128], esink[:, 0:1], ALU.add, ["psC1", "esink"], ["rec"])
        P.op("vector", lambda e: e.reciprocal(out=rec[:, 0:128], in_=rec[:, 0:128]), reads=["rec"], writes=["rec"])
        j4 = i % 4
        TT(P, "vector", ydst[:, j4 * 128:(j4 + 1) * 128], psC[0][:, 0:128], rec[:, 0:128], ALU.mult, ["psC0", "rec"], ["ydst"])
        if j4 == 3:
            q0 = (i - 3) * 128
            DMA(P, "sync", ydT[:, q0:q0 + 512], ydst[:], reads=["ydst"], is_out=True)

    qT = GA[:, 0:S_]
    kT = GB[:, :]
    vv = GC[:, :].rearrange("p (t d) -> p t d", d=128)

    def attn_loop(kind, out_dram):
        nonlocal npb
        for c in range(S_ // 512):
            q0 = 512 * c
            ng = 4 * (c + 1)
            for g in range(ng):
                K0 = 128 * g
                dlt = q0 - K0
                S = psS[npb % 2]
                sk = "psS%d" % (npb % 2)
                pb = Pb[npb % 3]
                pbk = "Pb%d" % (npb % 3)
                npb += 1
                bias = 0.0
                if kind == "moba":
                    near = (dlt - 127) < THR31
                    MM(P, S[:], kT[:, K0:K0 + 128], qT[:, q0:q0 + 512], True, False, ["GA", "GB"], [sk], sig=False)
                    n = g // 2
                    MM(P, S[:], sel[0:32, n * 128:(n + 1) * 128], GD[0:32, q0:q0 + 512], False, not near, ["sel", "GD"], [sk], sig=not near)
                    if near:
                        MM(P, S[:], ident_b[:], trev(TA, dlt + 384, 512), False, True, ["ident_b", "TA"], [sk])
                    else:
                        bias = csts[:, 0:1]
                    ACT(P, pb[:], S[:], AF.Exp, [sk, "csts"], [pbk], bias=bias)
                else:
                    diag = g >= 4 * c
                    MM(P, S[:], kT[:, K0:K0 + 128], qT[:, q0:q0 + 512], True, False, ["GA", "GB"], [sk], sig=False)
                    MM(P, S[:], ones_b[0:3, :], GD[0:3, q0:q0 + 512], False, not diag, ["ones_b", "GD"], [sk], sig=not diag)
                    if diag:
                        MM(P, S[:], ident_b[:], trev(TC, dlt + 384, 512), False, True, ["ident_b", "TC"], [sk])
                    ACT(P, pb[:], S[:], AF.Exp, [sk, "cptm"], [pbk], bias=cptm[:, g:g + 1])
                MM(P, psO[:], vv[:, g, :], pb[:], g == 0, g == ng - 1, ["GC", pbk], ["psO"])
                MM(P, psD[:], ones_b[:], pb[:], g == 0, g == ng - 1, ["ones_b", pbk], ["psD"])
            P.op("vector", lambda e: e.reciprocal(out=rec[:], in_=psD[:]), reads=["psD"], writes=["rec"])
            o = ost[c % 2]
            okk = "ost%d" % (c % 2)
            TT(P, "vector", o[:], psO[:], rec[:], ALU.mult, ["psO", "rec"], [okk])
            DMA(P, "sync", out_dram[:, q0:q0 + 512], o[:], reads=[okk], is_out=True)

    DMA(P, "sync", qT, aqT, writes=["GA"])
    DMA(P, "sync", kT, akT, writes=["GB"])
    DMA(P, "sync", vv, av.rearrange("(t p) d -> p t d", p=128), writes=["GC"])
    sel = cx.sb("sel", [32, 32 * 128], BF16)
    MEMSET(P, "gpsimd", sel[:], 1.0, ["sel"])
    P.op("gpsimd", lambda e: e.affine_select(out=sel[:], in_=sel[:], pattern=[[1, 32], [0, 128]], compare_op=ALU.is_equal,
                                             fill=0.0, base=0, channel_multiplier=-1), reads=["sel"], writes=["sel"])
    ksum = cx.sb("ksum", [128, 32], F32)
    kmb = cx.sb("kmb", [128, 32], BF16)
    P.op("vector", lambda e: e.reduce_sum(out=ksum[:], in_=kT.rearrange("p (n k) -> p n k", k=256), axis=AX.X), reads=["GB"], writes=["ksum"])
    TS(P, "vector", kmb[:], ksum[:], 1.0 / 256.0, ALU.mult, ["ksum"], ["kmb"])
    scsb = cx.sb("scsb", [128, 32], F32)
    mx = cx.sb("mx", [128, 8], F32)
    mb = cx.sb("mb", [128, 32], F32)
    MEMSET(P, "vector", scsb[:], -1e30, ["scsb"])
    for i in range(NT):
        own = i // 2
        MM(P, psM[:, 0:32], qT[:, i * 128:(i + 1) * 128], kmb[:], True, True, ["GA", "kmb"], ["psM"])
        if own > 0:
            CP(P, "vector", scsb[:, 0:own], psM[:, 0:own], ["psM"], ["scsb"])
        P.op("vector", lambda e: e.max(out=mx[:], in_=scsb[:]), reads=["scsb"], writes=["mx"])
        TS(P, "vector", mb[:], scsb[:], mx[:, 2:3], ALU.is_ge, ["scsb", "mx"], ["mb"])
        TS(P, "vector", mb[:], mb[:], -1.0, ALU.add, ["mb"], ["mb"], s2=-NEG, op1=ALU.mult)
        MEMSET(P, "vector", mb[:, own:own + 1], 0.0, ["mb"])
        TR(P, psM[0:32, 128:256], mb[:, :], ident_f[:], ["mb", "ident_f"], ["psM"])
        CP(P, "scalar", GD[0:32, i * 128:(i + 1) * 128], psM[0:32, 128:256], ["psM"], ["GD"])
    attn_loop("moba", yaT)

    brow = GA[0:1, :].bitcast(F32)
    crow = GD[0:1, :].bitcast(F32)
    DMA(P, "sync", brow, bfr, writes=["GA"])
    nfb = cx.sb("nfb", [1, 1], F32)
    TS(P, "vector", nfb[:], csts[0:1, 1:2], -1.0, ALU.mult, ["csts"], ["nfb"])
    ACT(P, brow, brow, AF.Exp, ["GA", "nfb"], ["GA"], bias=nfb[0:1, 0:1], scale=-1.0)
    ACT(P, brow, brow, AF.Ln, ["GA"], ["GA"], bias=1.0)
    P.op("vector", lambda e: e.tensor_tensor_scan(out=crow, data0=brow, data1=brow, initial=0.0, op0=ALU.add, op1=ALU.bypass),
         reads=["GA"], writes=["GD"])
    DMA(P, "sync", cps.rearrange("(a n) -> a n", a=1), crow, reads=["GD"], writes=["cps"])
    DMA(P, "sync", qT, bqT, writes=["GA"])
    DMA(P, "sync", kT, bkT, writes=["GB"])
    DMA(P, "sync", vv, bv.rearrange("(t p) d -> p t d", p=128), writes=["GC"])
    c64 = cx.sb("c64", [64, 128], F32)
    DMA(P, "sync", c64[:], cps.rearrange("(t p) -> t p", p=128), reads=["cps"], writes=["c64"])
    TR(P, psM[:, 256:320], c64[:, :], ident_f[0:64, 0:64], ["c64", "ident_f"], ["psM"])
    cptm = cx.sb("cptm", [128, 64], F32)
    CP(P, "vector", cptm[:], psM[:, 256:320], ["psM"], ["cptm"])
    ng_ = cx.sb("ng_", [64, 128], F32)
    tf = cx.sb("tf", [64, 128], F32)
    sp3 = cx.sb("sp3", [64, 3, 128], BF16)
    TS(P, "vector", ng_[:], c64[:], -1.0, ALU.mult, ["c64"], ["ng_"])
    for i3 in range(3):
        CP(P, "vector", sp3[:, i3, :], ng_[:], ["ng_"], ["sp3"])
        if i3 < 2:
            CP(P, "vector", tf[:], sp3[:, i3, :], ["sp3"], ["tf"])
            TT(P, "vector", ng_[:], ng_[:], tf[:], ALU.subtract, ["ng_", "tf"], ["ng_"])
    for i3 in range(3):
        DMA(P, "sync", cq3[i3].rearrange("(t p) -> t p", p=128), sp3[:, i3, :], reads=["sp3"], writes=["cq3"])
    DMA(P, "sync", GD[0:3, 0:S_], cq3, reads=["cq3"], writes=["GD"])
    attn_loop("fox", ybT)
    return cx.finish()


def inputs_B(d, l, b, g, A_outs):
    def fm(ch):
        return np.ascontiguousarray(np.concatenate([A_outs[j]["o_fm"][ch] for j in range(4)], axis=1))
    tm = np.concatenate([A_outs[j]["o_tm"] for j in range(4)], axis=0)
    bf = np.concatenate([A_outs[j]["o_bf"][g] for j in range(4)], axis=0)
    dqc = fm(20 + g)
    dkc = fm(24)
    kvh = g // 2
    oh, _ = toeplitz_consts()
    rb = np.asarray(d['rel_bias'], np.float32)
    rsel = np.zeros((33, 4), np.float32)
    rsel[0:32, 0] = rb[:, g]
    rsel[0:32, 1] = rb[:, 4 + 2 * g]
    rsel[0:32, 2] = rb[:, 4 + 2 * g + 1]
    rsel[32, :] = NEG
    cst = np.zeros((128, 8), np.float32)
    cst[:, 0] = rb[31, g]
    cst[:, 1] = d['fox_bias'][l][g]
    cst[:, 2] = d['sinks'][l][2 * g]
    cst[:, 3] = d['sinks'][l][2 * g + 1]
    cwv = np.zeros((128, 32), np.float32)
    cwv[:, 0:31] = d['conv_w'][l][:, 128 * g:128 * (g + 1)].T
    cwv[:, 31] = d['conv_b'][l][128 * g:128 * (g + 1)]
    return {
        "aqT": fm(0 + g), "akT": fm(4 + g), "av": np.ascontiguousarray(tm[:, 128 * g:128 * (g + 1)]),
        "bqT": fm(8 + g), "bkT": fm(12 + g), "bv": np.ascontiguousarray(tm[:, 512 + 128 * g:512 + 128 * (g + 1)]),
        "bfr": np.ascontiguousarray(bf.reshape(1, SEQ).astype(np.float32)),
        "hgT": fm(16 + g),
        "dqT": np.ascontiguousarray(dqc.reshape(2, 64, SEQ).transpose(1, 0, 2)),
        "dkT": np.ascontiguousarray(dkc[64 * kvh:64 * (kvh + 1)]),
        "dv": np.ascontiguousarray(tm[:, 1024 + 64 * kvh:1024 + 64 * (kvh + 1)]),
        "Rsel": rsel, "OH": oh, "cst": cst, "convw": cwv,
    }


C_STAGES = 99
C_HALVES = 2


def build_C():
    cx = Ctx()
    P = cx.P
    H = 1024
    xT = cx.dram_in("xT", [D, TOK], F32)
    cT = cx.dram_in("cT", [128, 16], F32)
    wmod = cx.dram_in("wmodC", [D, 8192], F32)
    vec = cx.dram_in("vecC", [128, 120], F32)
    hTd = cx.dram_in("hT", [16, 128, TOK], BF16)
    yabd = cx.dram_in("yabd", [3, 512, TOK], BF16)
    ycp = cx.dram_in("ycp", [512, TOK], F32)
    w_in = cx.dram_in("w_inG", [D, 4 * D], F32)
    w_br = cx.dram_in("w_br", [4, 512, D], F32)
    w_out = cx.dram_in("w_out", [D, D], F32)
    w_g = cx.dram_in("w_g", [D, DFF], F32)
    w_u = cx.dram_in("w_u", [D, DFF], F32)
    w_d = cx.dram_in("w_d", [DFF, D], F32)
    xoT = cx.dram_out("xoT", [D, TOK], F32)
    xmid = cx.dram_tmp("xmid", [D, TOK], F32)

    ones_b = cx.make_const("ones_bf", [128, 128], BF16, 1.0)
    ones_f = cx.make_const("ones_f", [128, 128], F32, 1.0)
    vecs = cx.sb("vecs", [128, 120], F32)
    DMA(P, "sync", vecs[:], vec, writes=["vecs"])
    WP = [cx.sb("WP%d" % i, [128, 6144], BF16) for i in range(2)]
    nwp = [0]

    def wslot():
        i = nwp[0] % 2
        nwp[0] += 1
        return WP[i], "WP%d" % i

    psG = [cx.ps("psG%d" % i, [128, 512]) for i in range(2)]
    psZ = [cx.ps("psZ%d" % i, [128, 512]) for i in range(2)]
    psT = [cx.ps("psT%d" % i, [128, 512]) for i in range(2)]
    psm = cx.ps("psm", [128, 512])

    modT = cx.sb("modT", [128, 64], F32)
    wb = [(WP[i][:, 0:4096].rearrange("p (k n) -> p k n", k=16), "WP%d" % i) for i in range(2)]
    emit_mod(cx, cT, wmod, 8192, vecs[:, 0:64], "vecs", modT, "modT", wb, psm, "psm", bw=256)
    nwp[0] = 0
    Gm = cx.sb("Gm", [128, 16], F32)
    A2 = cx.sb("A2", [128, 16], F32)
    Gf = cx.sb("Gf", [128, 16], F32)
    TT(P, "vector", Gm[:], modT[:, 0:16], vecs[:, 64:80], ALU.mult, ["modT", "vecs"], ["Gm"])
    TS(P, "vector", A2[:], modT[:, 32:48], 1.0, ALU.add, ["modT"], ["A2"])
    TT(P, "vector", A2[:], A2[:], vecs[:, 80:96], ALU.mult, ["A2", "vecs"], ["A2"])
    TT(P, "vector", Gf[:], modT[:, 48:64], vecs[:, 96:112], ALU.mult, ["modT", "vecs"], ["Gf"])
    B2 = modT[:, 16:32]

    hTh = cx.sb("hTh", [128, 16, H], BF16)
    X = cx.sb("X", [128, 44, H], BF16)
    Y = cx.sb("Y", [128, 16, H], BF16)
    rstd = cx.sb("rstd", [128, 512], F32)
    mean = cx.sb("mean", [128, 512], F32)
    xs = [cx.sb("xs%d" % i, [128, 512], F32) for i in range(4)]
    tmp = [cx.sb("tmp%d" % i, [128, 512], F32) for i in range(3)]
    sqb = [cx.sb("sqb%d" % i, [128, 512], BF16) for i in range(2)]
    acc = [cx.sb("acc%d" % i, [128, 512], F32) for i in range(2)]
    cnt = {"xs": 0, "tmp": 0, "sq": 0}

    def rot(name, lst):
        i = cnt[name] % len(lst)
        cnt[name] += 1
        return lst[i], "%s%d" % (name if name != "sq" else "sqb", i)

    def rstd_from(ps, pk, eps, scale):
        ACT(P, rstd[:], ps[:], AF.Sqrt, [pk], ["rstd"], bias=eps, scale=scale)
        P.op("vector", lambda e: e.reciprocal(out=rstd[:], in_=rstd[:]), reads=["rstd"], writes=["rstd"])

    for half in range(C_HALVES):
        T0 = half * H
        if C_STAGES < 1:
            break
        for k in range(16):
            DMA(P, "sync", hTh[:, k, :], hTd[k][:, T0:T0 + H], writes=["hTh%d" % k])
        for bi, slot in ((0, 0), (1, 1), (2, 3)):
            for kc in range(4):
                DMA(P, "sync", X[:, slot * 4 + kc, :], yabd[bi][kc * 128:(kc + 1) * 128, T0:T0 + H], writes=["X%d" % (slot * 4 + kc)])
        for tc in range(2):
            ts_ = slice(tc * 512, (tc + 1) * 512)
            xc = []
            for kc in range(4):
                x_, xk = xs[kc], "xs%d" % kc
                DMA(P, "sync", x_[:], ycp[kc * 128:(kc + 1) * 128, T0 + tc * 512:T0 + (tc + 1) * 512], writes=[xk])
                t_, tk = rot("tmp", tmp)
                ACT(P, t_[:], x_[:], AF.Square, [xk], [tk])
                MM(P, psT[0][:], ones_f[:], x_[:], kc == 0, kc == 3, ["ones_f", xk], ["psT0"])
                MM(P, psT[1][:], ones_f[:], t_[:], kc == 0, kc == 3, ["ones_f", tk], ["psT1"])
                xc.append((x_, xk))
            ACT(P, mean[:], psT[0][:], AF.Copy, ["psT0"], ["mean"], scale=1.0 / 512)
            t_, tk = rot("tmp", tmp)
            TT(P, "vector", t_[:], mean[:], mean[:], ALU.mult, ["mean"], [tk])
            P.op("vector", lambda e, t_=t_: e.scalar_tensor_tensor(out=t_[:], in0=psT[1][:], scalar=1.0 / 512, in1=t_[:], op0=ALU.mult, op1=ALU.subtract),
                 reads=["psT1", tk], writes=[tk])
            ACT(P, rstd[:], t_[:], AF.Sqrt, [tk], ["rstd"], bias=LN_EPS, scale=1.0)
            P.op("vector", lambda e: e.reciprocal(out=rstd[:], in_=rstd[:]), reads=["rstd"], writes=["rstd"])
            for kc in range(4):
                x_, xk = xc[kc]
                t_, tk = rot("tmp", tmp)
                TT(P, "vector", t_[:], x_[:], mean[:], ALU.subtract, [xk, "mean"], [tk])
                TT(P, "vector", t_[:], t_[:], rstd[:], ALU.mult, [tk, "rstd"], [tk])
                ACT(P, X[:, 8 + kc, ts_], t_[:], AF.Silu, [tk, "vecs"], ["X%d" % (8 + kc)], bias=vecs[:, 116 + kc:117 + kc], scale=vecs[:, 112 + kc:113 + kc])
        if C_STAGES < 2:
            continue
        for fo in range(16):
            for bi in range(4):
                wt, wk = wslot()
                wgv = wt[:, 0:2048].rearrange("p (k n) -> p k n", k=16)
                wbv = wt[:, 2048:2560].rearrange("p (k n) -> p k n", k=4)
                DMA(P, "gpsimd", wgv, wview(w_in, bi * D + fo * 128, 128, 16), writes=[wk])
                DMA(P, "gpsimd", wbv, wview(w_br[bi], fo * 128, 128, 4), writes=[wk])
                for tc in range(2):
                    ts_ = slice(tc * 512, (tc + 1) * 512)
                    for k in range(16):
                        MM(P, psG[tc][:], wgv[:, k, :], hTh[:, k, ts_], k == 0, k == 15, [wk, "hTh%d" % k], ["psG%d" % tc], sig=(k == 15))
                    for kc in range(4):
                        MM(P, psZ[tc][:], wbv[:, kc, :], X[:, bi * 4 + kc, ts_], kc == 0, kc == 3, [wk, "X%d" % (bi * 4 + kc)], ["psZ%d" % tc], sig=(kc == 3))
                    t_, tk = rot("tmp", tmp)
                    ACT(P, t_[:], psG[tc][:], AF.Sigmoid, ["psG%d" % tc], [tk])
                    if bi == 0:
                        TT(P, "vector", acc[tc][:], t_[:], psZ[tc][:], ALU.mult, [tk, "psZ%d" % tc], ["acc%d" % tc])
                    else:
                        TT(P, "vector", t_[:], t_[:], psZ[tc][:], ALU.mult, [tk, "psZ%d" % tc], [tk])
                        if bi < 3:
                            TT(P, "vector", acc[tc][:], acc[tc][:], t_[:], ALU.add, [tk, "acc%d" % tc], ["acc%d" % tc])
                        else:
                            TT(P, "vector", X[:, 16 + fo, ts_], acc[tc][:], t_[:], ALU.add, [tk, "acc%d" % tc], ["X%d" % (16 + fo)])
        if C_STAGES < 3:
            continue
        for fo in range(16):
            wt, wk = wslot()
            wv = wt[:, 0:2048].rearrange("p (k n) -> p k n", k=16)
            DMA(P, "gpsimd", wv, wview(w_out, fo * 128, 128, 16), writes=[wk])
            for tc in range(2):
                ts_ = slice(tc * 512, (tc + 1) * 512)
                for k in range(16):
                    MM(P, psG[tc][:], wv[:, k, :], X[:, 16 + k, ts_], k == 0, k == 15, [wk, "X%d" % (16 + k)], ["psG%d" % tc], sig=(k == 15))
                CP(P, "vector", Y[:, fo, ts_], psG[tc][:], ["psG%d" % tc], ["Y%d" % fo])
                s_, sk = rot("sq", sqb)
                ACT(P, s_[:], Y[:, fo, ts_], AF.Square, ["Y%d" % fo], [sk])
                MM(P, psT[tc][:], ones_b[:], s_[:], fo == 0, fo == 15, ["ones_bf", sk], ["psT%d" % tc])
        if C_STAGES < 4:
            continue
        for tc in range(2):
            ts_ = slice(tc * 512, (tc + 1) * 512)
            g0 = T0 + tc * 512
            rstd_from(psT[tc], "psT%d" % tc, RMS_EPS, 1.0 / D)
            for k in range(16):
                x_, xk = rot("xs", xs)
                DMA(P, "sync", x_[:], xT[k * 128:(k + 1) * 128, g0:g0 + 512], writes=[xk])
                t_, tk = rot("tmp", tmp)
                TT(P, "vector", t_[:], Y[:, k, ts_], rstd[:], ALU.mult, ["Y%d" % k, "rstd"], [tk])
                P.op("vector", lambda e, t_=t_, x_=x_, k=k: e.scalar_tensor_tensor(out=x_[:], in0=t_[:], scalar=Gm[:, k:k + 1], in1=x_[:], op0=ALU.mult, op1=ALU.add),
                     reads=[tk, xk, "Gm"], writes=[xk])
                DMA(P, "sync", xmid[k * 128:(k + 1) * 128, g0:g0 + 512], x_[:], reads=[xk], writes=["xmid%d_%d" % (k, g0)])
                s_, sk = rot("sq", sqb)
                ACT(P, s_[:], x_[:], AF.Square, [xk], [sk])
                MM(P, psZ[tc][:], ones_b[:], s_[:], k == 0, k == 15, ["ones_bf", sk], ["psZ%d" % tc])
            rstd_from(psZ[tc], "psZ%d" % tc, RMS_EPS, 1.0 / D)
            for k in range(16):
                x_, xk = rot("xs", xs)
                DMA(P, "sync", x_[:], xmid[k * 128:(k + 1) * 128, g0:g0 + 512], reads=["xmid%d_%d" % (k, g0)], writes=[xk])
                t_, tk = rot("tmp", tmp)
                TT(P, "vector", t_[:], x_[:], rstd[:], ALU.mult, [xk, "rstd"], [tk])
                ACT(P, hTh[:, k, ts_], t_[:], AF.Identity, [tk, "A2", "modT"], ["hTh%d" % k], bias=B2[:, k:k + 1], scale=A2[:, k:k + 1])
        if C_STAGES < 5:
            continue
        for f in range(DFF // 128):
            wt, wk = wslot()
            wgv = wt[:, 0:2048].rearrange("p (k n) -> p k n", k=16)
            wuv = wt[:, 2048:4096].rearrange("p (k n) -> p k n", k=16)
            DMA(P, "gpsimd", wgv, wview(w_g, f * 128, 128, 16), writes=[wk])
            DMA(P, "gpsimd", wuv, wview(w_u, f * 128, 128, 16), writes=[wk])
            for tc in range(2):
                ts_ = slice(tc * 512, (tc + 1) * 512)
                for k in range(16):
                    MM(P, psG[tc][:], wgv[:, k, :], hTh[:, k, ts_], k == 0, k == 15, [wk, "hTh%d" % k], ["psG%d" % tc], sig=(k == 15))
                for k in range(16):
                    MM(P, psZ[tc][:], wuv[:, k, :], hTh[:, k, ts_], k == 0, k == 15, [wk, "hTh%d" % k], ["psZ%d" % tc], sig=(k == 15))
                t_, tk = rot("tmp", tmp)
                ACT(P, t_[:], psG[tc][:], AF.Silu, ["psG%d" % tc], [tk])
                TT(P, "vector", X[:, f, ts_], t_[:], psZ[tc][:], ALU.mult, [tk, "psZ%d" % tc], ["X%d" % f])
        if C_STAGES < 6:
            continue
        for fo in range(16):
            wt, wk = wslot()
            wv = wt[:, 0:44 * 128].rearrange("p (k n) -> p k n", k=44)
            DMA(P, "gpsimd", wv[:, 0:22, :], wview(w_d, fo * 128, 128, 44)[:, 0:22, :], writes=[wk])
            DMA(P, "gpsimd", wv[:, 22:44, :], wview(w_d, fo * 128, 128, 44)[:, 22:44, :], writes=[wk])
            for tc in range(2):
                ts_ = slice(tc * 512, (tc + 1) * 512)
                for f in range(44):
                    MM(P, psG[tc][:], wv[:, f, :], X[:, f, ts_], f == 0, f == 43, [wk, "X%d" % f], ["psG%d" % tc], sig=(f == 43))
                CP(P, "vector", Y[:, fo, ts_], psG[tc][:], ["psG%d" % tc], ["Y%d" % fo])
                s_, sk = rot("sq", sqb)
                ACT(P, s_[:], Y[:, fo, ts_], AF.Square, ["Y%d" % fo], [sk])
                MM(P, psT[tc][:], ones_b[:], s_[:], fo == 0, fo == 15, ["ones_bf", sk], ["psT%d" % tc])
        if C_STAGES < 7:
            continue
        for tc in range(2):
            ts_ = slice(tc * 512, (tc + 1) * 512)
            g0 = T0 + tc * 512
            rstd_from(psT[tc], "psT%d" % tc, RMS_EPS, 1.0 / D)
            for k in range(16):
                x_, xk = rot("xs", xs)
                DMA(P, "sync", x_[:], xmid[k * 128:(k + 1) * 128, g0:g0 + 512], reads=["xmid%d_%d" % (k, g0)], writes=[xk])
                t_, tk = rot("tmp", tmp)
                TT(P, "vector", t_[:], Y[:, k, ts_], rstd[:], ALU.mult, ["Y%d" % k, "rstd"], [tk])
                P.op("vector", lambda e, t_=t_, x_=x_, k=k: e.scalar_tensor_tensor(out=x_[:], in0=t_[:], scalar=Gf[:, k:k + 1], in1=x_[:], op0=ALU.mult, op1=ALU.add),
                     reads=[tk, xk, "Gf"], writes=[xk])
                DMA(P, "sync", xoT[k * 128:(k + 1) * 128, g0:g0 + 512], x_[:], reads=[xk], is_out=True)
    return cx.finish()


def inputs_C(d, l, b, j, xT, A_out, B_outs):
    ts_ = slice(j * TOK, (j + 1) * TOK)
    ya = np.concatenate([B_outs[g]["yaT"][:, ts_] for g in range(4)], axis=0)
    yb = np.concatenate([B_outs[g]["ybT"][:, ts_] for g in range(4)], axis=0)
    yd = np.concatenate([B_outs[g]["ydT"][:, ts_] for g in range(4)], axis=0)
    yc = np.concatenate([B_outs[g]["ycT"][:, ts_] for g in range(4)], axis=0)
    vec = np.concatenate([
        colmajor(d['b_mod'][l][4096:12288]), colmajor(d['mix_norm_post'][l]), colmajor(d['ffn_norm_pre'][l]),
        colmajor(d['ffn_norm_post'][l]), colmajor(d['conv_ln_g'][l]), colmajor(d['conv_ln_b'][l])], axis=1).astype(np.float32)
    return {
        "xT": np.ascontiguousarray(xT, dtype=np.float32),
        "cT": colmajor(d['c'][b]).astype(np.float32),
        "wmodC": np.ascontiguousarray(d['w_mod'][l][:, 4096:12288]),
        "vecC": np.ascontiguousarray(vec),
        "hT": np.ascontiguousarray(A_out["o_hT"]),
        "yabd": np.ascontiguousarray(np.stack([ya, yb, yd], axis=0)),
        "ycp": np.ascontiguousarray(yc, dtype=np.float32),
        "w_inG": np.ascontiguousarray(d['w_in'][l][:, O_GL:]),
        "w_br": np.ascontiguousarray(np.stack([d['w_branch_a'][l], d['w_branch_b'][l], d['w_branch_c'][l], d['w_branch_d'][l]], axis=0)),
        "w_out": np.ascontiguousarray(d['w_out'][l]),
        "w_g": np.ascontiguousarray(d['w_ffn_gate'][l]),
        "w_u": np.ascontiguousarray(d['w_ffn_up'][l]),
        "w_d": np.ascontiguousarray(d['w_ffn_down'][l]),
    }


_PROGS = {}


def _prog(name):
    if name not in _PROGS:
        _PROGS[name] = {"A": build_A, "B": build_B, "C": build_C}[name]()
    return _PROGS[name]


def kernel(**inputs):
    d = {k: np.asarray(v) for k, v in inputs.items()}
    cores = list(range(8))
    xTs = [np.ascontiguousarray(d['x'][b, j * TOK:(j + 1) * TOK].T) for b in range(NB) for j in range(4)]
    for l in range(DEPTH):
        insA = [inputs_A(d, l, b, xTs[b * 4 + j]) for b in range(NB) for j in range(4)]
        resA = run_bass_kernel_spmd(_prog("A"), insA, core_ids=cores).results
        del insA
        insB = [inputs_B(d, l, b, g, resA[b * 4:(b + 1) * 4]) for b in range(NB) for g in range(4)]
        resB = run_bass_kernel_spmd(_prog("B"), insB, core_ids=cores).results
        del insB
        insC = [inputs_C(d, l, b, j, xTs[b * 4 + j], resA[b * 4 + j], resB[b * 4:(b + 1) * 4]) for b in range(NB) for j in range(4)]
        resC = run_bass_kernel_spmd(_prog("C"), insC, core_ids=cores).results
        del insC
        xTs = [np.asarray(resC[i]["xoT"]) for i in range(8)]
    out = np.empty((NB, SEQ, D), np.float32)
    for b in range(NB):
        for j in range(4):
            out[b, j * TOK:(j + 1) * TOK, :] = xTs[b * 4 + j].T
    return out
```
